# Optimizing a Trainium2 kernel written in Bass

```python
import math
import jax, jax.numpy as jnp
from jax import lax
import numpy as np

D_MODEL = 2048
BATCH = 2
SEQ = 16384
DEPTH = 1

N_MEM = 256
XA_HEADS = 4
XA_DH = D_MODEL // XA_HEADS
RET_HEADS = 8
RET_DK = 128
RET_DV = 128
DN_HEADS = 8
DN_DK = 128
DN_DV = 128
CONV_W = 4
CHUNK = 128
ROPE_BASE = 10000.0
MAX_POS_OFFSET = 4096
EPS = 1e-6
N_GROUPS = 4
EXP_PER_GROUP = 8
N_EXPERTS = N_GROUPS * EXP_PER_GROUP
TOP_K = 2
D_FF_EXPERT = 1024
MOE_BLOCK = 128
RET_QK_W = RET_HEADS * RET_DK
RET_V_W = RET_HEADS * RET_DV
DN_QK_W = DN_HEADS * DN_DK
DN_V_W = DN_HEADS * DN_DV
DN_QKV_W = 2 * DN_QK_W + DN_V_W
MIX_W = RET_V_W + DN_V_W
IN_DIM = 2 * RET_QK_W + 2 * RET_V_W + DN_QKV_W + DN_V_W + 2 * DN_HEADS

kernel_name = "hymba_retention_gdn_hmoe_block"


def rmsnorm(x, w):
    xf = x.astype(jnp.float32)
    y = xf * lax.rsqrt(jnp.mean(xf * xf, axis=-1, keepdims=True) + EPS)
    return (y * w.astype(jnp.float32)).astype(x.dtype)


def l2norm(x):
    return x * lax.rsqrt(jnp.sum(x * x, axis=-1, keepdims=True) + EPS)


def rotary(x, positions):
    half = x.shape[-1] // 2
    inv = ROPE_BASE ** (-jnp.arange(half, dtype=jnp.float32) / half)
    ang = positions.astype(jnp.float32)[..., None] * inv
    cos = jnp.cos(ang)[:, :, None, :]
    sin = jnp.sin(ang)[:, :, None, :]
    x1, x2 = x[..., :half], x[..., half:]
    return jnp.concatenate([x1 * cos - x2 * sin, x1 * sin + x2 * cos], axis=-1)


def to_chunks(x):
    B, S, H, d = x.shape
    return x.reshape(B, S // CHUNK, CHUNK, H, d).transpose(1, 0, 3, 2, 4)


def scalars_to_chunks(x):
    B, S, H = x.shape
    return x.reshape(B, S // CHUNK, CHUNK, H).transpose(1, 0, 3, 2)


def from_chunks(o):
    N, B, H, C, d = o.shape
    return o.transpose(1, 0, 3, 2, 4).reshape(B, N * C, H, d)


def causal_depthwise_conv(x, w):
    return lax.conv_general_dilated(
        x, w[:, None, :].astype(x.dtype), window_strides=(1,), padding=[(CONV_W - 1, 0)],
        dimension_numbers=("NWC", "WIO", "NWC"), feature_group_count=x.shape[-1])


def retention(q, k, v, positions):
    B = q.shape[0]
    q = rotary(q, positions)
    k = rotary(k, positions) * (RET_DK ** -0.5)
    log_gamma = jnp.log1p(-jnp.exp2(-5.0 - jnp.arange(RET_HEADS, dtype=jnp.float32)))
    idx = jnp.arange(CHUNK, dtype=jnp.float32)
    rel = idx[:, None] - idx[None, :]
    causal = rel >= 0
    d_intra = jnp.where(causal, jnp.exp(log_gamma[:, None, None] * jnp.where(causal, rel, 0.0)), 0.0)
    q_dec = jnp.exp(log_gamma[:, None] * (idx + 1.0))[None, :, :, None]
    k_dec = jnp.exp(log_gamma[:, None] * (CHUNK - 1.0 - idx))[None, :, :, None]
    chunk_dec = jnp.exp(log_gamma * CHUNK)[None, :, None, None]

    def step(R, inp):
        qi, ki, vi = inp
        s = jnp.einsum("bhid,bhjd->bhij", qi, ki) * d_intra
        o = jnp.einsum("bhij,bhjv->bhiv", s, vi) + jnp.einsum("bhid,bhdv->bhiv", qi, R) * q_dec
        R = R * chunk_dec + jnp.einsum("bhjd,bhjv->bhdv", ki * k_dec, vi)
        return R, o

    R0 = jnp.zeros((B, RET_HEADS, RET_DK, RET_DV), jnp.float32)
    _, o = lax.scan(step, R0, (to_chunks(q), to_chunks(k), to_chunks(v)))
    return from_chunks(o)


def gated_delta_net(q, k, v, g_log, beta):
    B = q.shape[0]
    q = l2norm(q) * (DN_DK ** -0.5)
    k = l2norm(k)
    qc, kc, vc = to_chunks(q), to_chunks(k), to_chunks(v)
    gc = jnp.cumsum(scalars_to_chunks(g_log), axis=-1)
    bc = scalars_to_chunks(beta)
    idx = jnp.arange(CHUNK)
    causal = idx[:, None] >= idx[None, :]
    strict = idx[:, None] > idx[None, :]
    diff = gc[..., :, None] - gc[..., None, :]
    gam = jnp.where(causal, jnp.exp(jnp.where(causal, diff, 0.0)), 0.0)
    a = jnp.where(strict, jnp.einsum("nbhid,nbhjd->nbhij", kc, kc) * gam * bc[..., :, None], 0.0)
    eye = jnp.eye(CHUNK, dtype=jnp.float32)
    rhs = jnp.concatenate([vc * bc[..., None], kc * (bc * jnp.exp(gc))[..., None]], axis=-1)
    sol = lax.linalg.triangular_solve(a + eye, rhs, left_side=True, lower=True, unit_diagonal=True)
    u, w = sol[..., :DN_DV], sol[..., DN_DV:]
    qk = jnp.einsum("nbhid,nbhjd->nbhij", qc, kc) * gam
    q_dec = qc * jnp.exp(gc)[..., None]
    g_last = gc[..., -1]
    k_dec = kc * jnp.exp(g_last[..., None] - gc)[..., None]
    state_dec = jnp.exp(g_last)

    def step(S, inp):
        u_i, w_i, qk_i, q_i, k_i, sd = inp
        v_new = u_i - jnp.einsum("bhik,bhkv->bhiv", w_i, S)
        o = jnp.einsum("bhik,bhkv->bhiv", q_i, S) + jnp.einsum("bhij,bhjv->bhiv", qk_i, v_new)
        S = S * sd[..., None, None] + jnp.einsum("bhjk,bhjv->bhkv", k_i, v_new)
        return S, o

    S0 = jnp.zeros((B, DN_HEADS, DN_DK, DN_DV), jnp.float32)
    _, o = lax.scan(step, S0, (u, w, qk, q_dec, k_dec, state_dec))
    return from_chunks(o)


def token_mixer(xn, positions, w_in, conv_w, a_log, dt_bias, ret_gn_w, dn_norm_w, w_out):
    B, S, _ = xn.shape
    f32 = jnp.float32
    proj = xn @ w_in
    sizes = (RET_QK_W, RET_QK_W, RET_V_W, RET_V_W, DN_QKV_W, DN_V_W, DN_HEADS, DN_HEADS)
    splits = np.cumsum(sizes)[:-1].tolist()
    r_q, r_k, r_v, r_g, d_qkv, d_z, d_b, d_a = jnp.split(proj, splits, axis=-1)
    rq = r_q.astype(f32).reshape(B, S, RET_HEADS, RET_DK)
    rk = r_k.astype(f32).reshape(B, S, RET_HEADS, RET_DK)
    rv = r_v.astype(f32).reshape(B, S, RET_HEADS, RET_DV)
    ro = retention(rq, rk, rv, positions)
    mu = jnp.mean(ro, axis=-1, keepdims=True)
    var = jnp.mean(jnp.square(ro - mu), axis=-1, keepdims=True)
    ro = (ro - mu) * lax.rsqrt(var + EPS) * ret_gn_w.astype(f32).reshape(RET_HEADS, RET_DV)
    ro = ro.reshape(B, S, RET_V_W) * jax.nn.silu(r_g.astype(f32))
    d_qkv = jax.nn.silu(causal_depthwise_conv(d_qkv, conv_w)).astype(f32)
    dq, dk, dv = jnp.split(d_qkv, [DN_QK_W, 2 * DN_QK_W], axis=-1)
    beta = jax.nn.sigmoid(d_b.astype(f32))
    g_log = -jnp.exp(a_log.astype(f32)) * jax.nn.softplus(d_a.astype(f32) + dt_bias.astype(f32))
    do = gated_delta_net(dq.reshape(B, S, DN_HEADS, DN_DK), dk.reshape(B, S, DN_HEADS, DN_DK),
                         dv.reshape(B, S, DN_HEADS, DN_DV), g_log, beta)
    do = do * lax.rsqrt(jnp.mean(do * do, axis=-1, keepdims=True) + EPS) * dn_norm_w.astype(f32)
    do = do.reshape(B, S, DN_V_W) * jax.nn.silu(d_z.astype(f32))
    mix = jnp.concatenate([ro, do], axis=-1).astype(xn.dtype)
    return mix @ w_out


def memory_cross_attention(hn, memn, w_q, w_kv, w_o):
    B, S, D = hn.shape
    M = memn.shape[1]
    q = (hn @ w_q).reshape(B, S, XA_HEADS, XA_DH)
    k, v = jnp.split(memn @ w_kv, 2, axis=-1)
    k = k.reshape(B, M, XA_HEADS, XA_DH)
    v = v.reshape(B, M, XA_HEADS, XA_DH)
    s = jnp.einsum("bshd,bmhd->bhsm", q, k).astype(jnp.float32) * (XA_DH ** -0.5)
    p = jax.nn.softmax(s, axis=-1).astype(v.dtype)
    o = jnp.einsum("bhsm,bmhd->bshd", p, v).reshape(B, S, D)
    return o @ w_o


def hierarchical_moe(hn, w_group_router, b_group_router, w_expert_router, b_expert_router, w_gate, w_up, w_down):
    B, S, D = hn.shape
    T = B * S
    xt = hn.reshape(T, D)
    g_logits = (xt @ w_group_router).astype(jnp.float32) + b_group_router.astype(jnp.float32)
    g_prob = jax.nn.softmax(g_logits, axis=-1)
    g_sel = jnp.argmax(g_logits, axis=-1)
    g_w = jnp.take_along_axis(g_prob, g_sel[:, None], axis=-1)[:, 0]
    e_all = (xt @ w_expert_router).astype(jnp.float32) + b_expert_router.astype(jnp.float32)
    e_all = e_all.reshape(T, N_GROUPS, EXP_PER_GROUP)
    e_logits = jnp.take_along_axis(e_all, g_sel[:, None, None], axis=1)[:, 0]
    top_p, top_e = lax.top_k(jax.nn.softmax(e_logits, axis=-1), TOP_K)
    gate = g_w[:, None] * top_p / jnp.sum(top_p, axis=-1, keepdims=True)
    expert = g_sel[:, None].astype(jnp.int32) * EXP_PER_GROUP + top_e.astype(jnp.int32)
    A = T * TOP_K
    flat_e = expert.reshape(A)
    flat_tok = jnp.arange(A, dtype=jnp.int32) // TOP_K
    flat_gate = gate.reshape(A)
    order = jnp.argsort(flat_e)
    e_sorted, tok_sorted, gate_sorted = flat_e[order], flat_tok[order], flat_gate[order]
    counts = jax.ops.segment_sum(jnp.ones_like(flat_e), flat_e, num_segments=N_EXPERTS)
    start = jnp.cumsum(counts) - counts
    padded = (counts + MOE_BLOCK - 1) // MOE_BLOCK * MOE_BLOCK
    pend = jnp.cumsum(padded)
    pstart = pend - padded
    dest = pstart[e_sorted] + (jnp.arange(A, dtype=jnp.int32) - start[e_sorted])
    NB = -(-A // MOE_BLOCK) + N_EXPERTS
    P = NB * MOE_BLOCK
    buf = jnp.zeros((P, D), xt.dtype).at[dest].set(xt[tok_sorted])
    block_expert = jnp.minimum(
        jnp.searchsorted(pend, jnp.arange(NB, dtype=jnp.int32) * MOE_BLOCK, side="right"), N_EXPERTS - 1)

    def expert_block(args):
        xb, e = args
        hb = jax.nn.silu(xb @ w_gate[e]) * (xb @ w_up[e])
        return hb @ w_down[e]

    yb = lax.map(expert_block, (buf.reshape(NB, MOE_BLOCK, D), block_expert))
    y = yb.reshape(P, D)[dest] * gate_sorted[:, None].astype(xt.dtype)
    return jax.ops.segment_sum(y, tok_sorted, num_segments=T).reshape(B, S, D)


def setup_inputs(seed: int = 0) -> dict:
    key = jax.random.key(seed)
    ks = jax.random.split(key, 25)
    f32 = jnp.float32
    L = DEPTH

    def nrm(k, shape, scale):
        return jax.random.normal(k, shape, f32) * scale

    def gain(k, shape):
        return 1.0 + 0.02 * jax.random.normal(k, shape, f32)

    x = nrm(ks[0], (BATCH, SEQ, D_MODEL), 1.0)
    mem = nrm(ks[1], (BATCH, N_MEM, D_MODEL), 1.0)
    offset = jax.random.randint(ks[2], (BATCH, 1), 0, MAX_POS_OFFSET, dtype=jnp.int32)
    positions = (offset + jnp.arange(SEQ, dtype=jnp.int32)[None, :]).astype(jnp.int32)
    dt = jnp.exp(jax.random.uniform(ks[7], (L, DN_HEADS), f32, math.log(1e-3), math.log(1e-1)))
    return {
        "x": x,
        "mem": mem,
        "positions": positions,
        "norm_mix_w": gain(ks[3], (L, D_MODEL)),
        "w_in": nrm(ks[4], (L, D_MODEL, IN_DIM), D_MODEL ** -0.5),
        "dn_conv_w": nrm(ks[5], (L, CONV_W, DN_QKV_W), CONV_W ** -0.5),
        "dn_a_log": jnp.log(jax.random.uniform(ks[6], (L, DN_HEADS), f32, 1.0, 16.0)),
        "dn_dt_bias": dt + jnp.log(-jnp.expm1(-dt)),
        "ret_gn_w": gain(ks[8], (L, RET_V_W)),
        "dn_norm_w": gain(ks[9], (L, DN_DV)),
        "w_out": nrm(ks[10], (L, MIX_W, D_MODEL), MIX_W ** -0.5),
        "norm_xq_w": gain(ks[11], (L, D_MODEL)),
        "norm_mem_w": gain(ks[12], (L, D_MODEL)),
        "w_xq": nrm(ks[13], (L, D_MODEL, D_MODEL), D_MODEL ** -0.5),
        "w_xkv": nrm(ks[14], (L, D_MODEL, 2 * D_MODEL), D_MODEL ** -0.5),
        "w_xo": nrm(ks[15], (L, D_MODEL, D_MODEL), D_MODEL ** -0.5),
        "norm_moe_w": gain(ks[16], (L, D_MODEL)),
        "w_group_router": nrm(ks[17], (L, D_MODEL, N_GROUPS), D_MODEL ** -0.5),
        "b_group_router": nrm(ks[18], (L, N_GROUPS), 0.01),
        "w_expert_router": nrm(ks[19], (L, D_MODEL, N_EXPERTS), D_MODEL ** -0.5),
        "b_expert_router": nrm(ks[20], (L, N_EXPERTS), 0.01),
        "w_gate": nrm(ks[21], (L, N_EXPERTS, D_MODEL, D_FF_EXPERT), D_MODEL ** -0.5),
        "w_up": nrm(ks[22], (L, N_EXPERTS, D_MODEL, D_FF_EXPERT), D_MODEL ** -0.5),
        "w_down": nrm(ks[23], (L, N_EXPERTS, D_FF_EXPERT, D_MODEL), D_FF_EXPERT ** -0.5),
        "norm_final_w": gain(ks[24], (D_MODEL,)),
    }


def reference(x, mem, positions, norm_mix_w, w_in, dn_conv_w, dn_a_log, dn_dt_bias, ret_gn_w, dn_norm_w,
              w_out, norm_xq_w, norm_mem_w, w_xq, w_xkv, w_xo, norm_moe_w, w_group_router, b_group_router,
              w_expert_router, b_expert_router, w_gate, w_up, w_down, norm_final_w):
    h = x
    for l in range(DEPTH):
        h = h + token_mixer(rmsnorm(h, norm_mix_w[l]), positions, w_in[l], dn_conv_w[l], dn_a_log[l],
                            dn_dt_bias[l], ret_gn_w[l], dn_norm_w[l], w_out[l])
        h = h + memory_cross_attention(rmsnorm(h, norm_xq_w[l]), rmsnorm(mem, norm_mem_w[l]),
                                       w_xq[l], w_xkv[l], w_xo[l])
        h = h + hierarchical_moe(rmsnorm(h, norm_moe_w[l]), w_group_router[l], b_group_router[l],
                                 w_expert_router[l], b_expert_router[l], w_gate[l], w_up[l], w_down[l])
    return rmsnorm(h, norm_final_w)
```

```python
import math
import numpy as np
import ml_dtypes
import contextlib
import concourse.bass as bass
import concourse.mybir as mybir

F32 = mybir.dt.float32
BF16 = mybir.dt.bfloat16
I32 = mybir.dt.int32
AF = mybir.ActivationFunctionType
ALU = mybir.AluOpType

QUEUES = ("pe", "act", "dve", "pool", "sp")
N_DMA_SEMS = 24
SAME_ENGINE_SYNC = {"act", "dve", "pool"}


class Prog:
    def __init__(self, nc):
        self.nc = nc
        self.es = contextlib.ExitStack()
        self.lists = {q: [] for q in QUEUES}
        self.seq = {q: 0 for q in QUEUES}
        self.waited = {q: {} for q in QUEUES}
        self.sem = {q: self.es.enter_context(nc.semaphore("s_" + q)) for q in QUEUES}
        self.dsem = [self.es.enter_context(nc.semaphore("d%d" % i)) for i in range(N_DMA_SEMS)]
        self.dcnt = [0] * N_DMA_SEMS
        self.dnext = 0
        self.lastw = {}
        self.readers = {}
        self.ntile = 0
        self.nops = 0
        self.limit = None

    def sb(self, shape, dt, name=None):
        self.ntile += 1
        return self.es.enter_context(self.nc.sbuf_tensor(name or ("t%d" % self.ntile), list(shape), dt))

    def ps(self, shape, dt, name=None):
        self.ntile += 1
        return self.es.enter_context(self.nc.psum_tensor(name or ("p%d" % self.ntile), list(shape), dt))

    def _deps(self, q, reads, writes):
        deps = []
        for r in reads:
            if r in self.lastw:
                deps.append(self.lastw[r])
        for w in writes:
            if w in self.lastw:
                deps.append(self.lastw[w])
            deps.extend(self.readers.get(w, ()))
        waits = []
        need = {}
        for d in deps:
            if d[0] == "dma":
                key = ("dma", d[1])
                need[key] = max(need.get(key, 0), d[2])
            else:
                dq, ds = d
                if dq == q and q not in SAME_ENGINE_SYNC:
                    continue
                need[dq] = max(need.get(dq, 0), ds)
        wd = self.waited[q]
        for key, val in need.items():
            if wd.get(key, 0) >= val:
                continue
            wd[key] = val
            if isinstance(key, tuple):
                waits.append((self.dsem[key[1]], val))
            else:
                waits.append((self.sem[key], val))
        return waits

    def _commit(self, token, reads, writes):
        for r in reads:
            self.readers.setdefault(r, []).append(token)
        for w in writes:
            self.lastw[w] = token
            self.readers[w] = []

    def op(self, q, fn, reads=(), writes=()):
        self.nops += 1
        if self.limit is not None and self.nops > self.limit:
            return
        waits = self._deps(q, reads, writes)
        self.seq[q] += 1
        token = (q, self.seq[q])
        sem = self.sem[q]
        self.lists[q].append((waits, fn, sem, 1))
        self._commit(token, reads, writes)

    def dma(self, q, out, in_, reads=(), writes=(), **kw):
        i = self.dnext
        self.dnext = (self.dnext + 1) % N_DMA_SEMS
        waits = self._deps(q, reads, writes)
        prev = self.dcnt[i]
        if prev and self.waited[q].get(("dma", i), 0) < prev:
            self.waited[q][("dma", i)] = prev
            waits.append((self.dsem[i], prev))
        self.dcnt[i] += 16
        token = ("dma", i, self.dcnt[i])
        sem = self.dsem[i]
        self.lists[q].append((waits, lambda e: e.dma_start(out, in_, **kw), sem, 16))
        self._commit(token, reads, writes)

    def wait_all(self, q, keys):
        waits = self._deps(q, keys, ())
        self.lists[q].append((waits, None, None, 0))

    def emit(self):
        nc = self.nc
        engs = {"pe": "tensor", "act": "scalar", "dve": "vector", "pool": "gpsimd", "sp": "sync"}
        with nc.Block() as block:
            for q in QUEUES:
                lst = self.lists[q]

                def body(e, lst=lst):
                    for waits, fn, sem, inc in lst:
                        for s, v in waits:
                            e.wait_ge(s, v)
                        if fn is not None:
                            fn(e).then_inc(sem, inc)

                getattr(block, engs[q])(body)

    def close(self):
        self.es.close()

from concourse.bass_utils import run_bass_kernel_spmd

D = 2048

NT = 1284
NF = 768
EPS = 1e-6
TWO_PI = 2.0 * math.pi
C1 = 6.28125
C2 = TWO_PI - C1


def a_inputs(nc, NCH):
    T = NCH * 128
    di = {}

    def inp(name, shape, dt):
        di[name] = nc.dram_tensor(name, list(shape), dt, kind="ExternalInput").ap()

    inp("x", [T, D], F32)
    inp("posT", [128, NCH], I32)
    inp("w_tok", [D, NT], F32)
    inp("w_feat", [D, NF], F32)
    inp("nw", [128, 16], F32)
    inp("convw", [128, 24], F32)
    inp("smallc", [128, 16], F32)
    inp("gnw", [128, 256], F32)
    inp("dnw", [128, 128], F32)
    inp("identb", [128, 128], BF16)
    inp("cf", [128, 6, 128], F32)
    inp("mask2", [128, 256], F32)
    inp("invf8", [128, 512], F32)
    inp("phase8", [128, 512], F32)
    return di


def a_host_consts(h0_ret, NCH):
    c = {}
    c["identb"] = np.eye(128, dtype=np.float32).astype(ml_dtypes.bfloat16)
    j = np.arange(128)[:, None]
    i = np.arange(128)[None, :]
    cf = np.zeros((128, 6, 128), np.float32)
    cf[:, 0] = np.eye(128)
    cf[:, 1] = (i >= j)
    cf[:, 2] = (i > j)
    cf[:, 3] = np.where(i < j, -30000.0, 0.0)
    cf[:, 4] = 1.0
    c["cf"] = cf
    c["mask2"] = np.concatenate([cf[:, 1], cf[:, 1]], axis=1).astype(np.float32)
    inv = (np.float32(10000.0) ** (-np.arange(64, dtype=np.float32) / np.float32(64))).astype(np.float32)
    invf8 = np.broadcast_to(inv[None, None, None, :], (128, 2, 4, 64)).reshape(128, 512)
    c["invf8"] = np.ascontiguousarray(invf8, dtype=np.float32)
    ph = np.zeros((128, 2, 4, 64), np.float32)
    ph[:, 1] = math.pi / 2
    c["phase8"] = ph.reshape(128, 512)
    sm = np.zeros((128, 16), np.float32)
    idx = np.arange(128, dtype=np.float64)
    for hh in range(2):
        h = h0_ret + hh
        lg = math.log1p(-2.0 ** (-5.0 - h))
        sm[:, 4 + hh] = math.exp(lg * 128)
        sm[:, 6 + hh * 3 + 0] = np.exp(lg * (idx + 1.0))
        sm[:, 6 + hh * 3 + 1] = np.exp(-lg * (idx + 1.0)) * (128 ** -0.5)
        sm[:, 6 + hh * 3 + 2] = np.exp(lg * (127.0 - idx)) * (128 ** -0.5)
    c["smallc"] = sm
    return c


def build_A(p, di, mix_out, NCH):
    nc = p.nc
    wtok = p.sb([128, 16, NT], BF16)
    wfeat = p.sb([128, 16, NF], BF16)
    stg = p.sb([128, 2, NT], F32)
    xs = [p.sb([128, D], F32) for _ in range(2)]
    ub = p.sb([128, D], BF16)
    uT = p.sb([128, 16, 128], BF16)
    dg = p.sb([128, 24, 128], BF16)
    nw = p.sb([128, 16], F32)
    convw = p.sb([128, 24], F32)
    smallc = p.sb([128, 16], F32)
    gnw = p.sb([128, 256], F32)
    dnw = p.sb([128, 128], F32)
    identb = p.sb([128, 128], BF16)
    cf = p.sb([128, 6, 128], F32)
    mask2 = p.sb([128, 256], F32)
    invf8 = p.sb([128, 512], F32)
    phase8 = p.sb([128, 512], F32)
    posi = p.sb([128, NCH], I32)
    posf = p.sb([128, NCH], F32)
    nea = p.sb([128, 2], F32)
    identf, maskT, strictT, negT, onesf = (cf[:, k, :] for k in range(5))
    alog, dtb, cdec = smallc[:, 0:2], smallc[:, 2:4], smallc[:, 4:6]

    def dec(h, k):
        return smallc[:, 6 + h * 3 + k: 7 + h * 3 + k]

    st1 = p.sb([128, 8], F32)
    ang = [p.sb([128, 512], F32) for _ in range(4)]
    angi = p.sb([128, 512], I32)
    sc8 = p.sb([128, 512], F32)
    xc = [p.sb([128, 6, 132], BF16) for _ in range(2)]
    cT = p.sb([128, 6, 128], BF16)
    tmb = p.sb([128, 6, 128], BF16)
    sq = p.sb([128, 4, 128], F32)
    st2 = p.sb([128, 16], F32)
    qk = p.sb([128, 512], F32)
    rot = p.sb([128, 512], F32)
    tt = [p.sb([128, 256], F32) for _ in range(4)]
    rb = p.sb([128, 6, 128], BF16)
    rT = p.sb([128, 4, 128], BF16)
    sTm = p.sb([128, 256], BF16)
    vb = p.sb([128, 256], BF16)
    gs = p.sb([128, 256], F32)
    zs = p.sb([128, 256], F32)
    st3 = p.sb([128, 32], F32)
    GU = p.sb([128, 2, 128], F32)
    GUn = p.sb([128, 2, 128], F32)
    gamT = p.sb([128, 2, 128], F32)
    gamTs = p.sb([128, 2, 128], F32)
    knb = p.sb([128, 2, 128], BF16)
    qnb = p.sb([128, 2, 128], BF16)
    qgb = p.sb([128, 2, 128], BF16)
    kegb = p.sb([128, 2, 128], BF16)
    kdb = p.sb([128, 2, 128], BF16)
    T2 = p.sb([128, 6, 128], BF16)
    Nf = [p.sb([128, 2, 128], F32) for _ in range(2)]
    Bf = [p.sb([128, 2, 128], F32) for _ in range(2)]
    Qf = [p.sb([128, 2, 128], F32) for _ in range(2)]
    Qb = p.sb([128, 2, 128], BF16)
    qkTm = p.sb([128, 2, 128], BF16)
    wTb = p.sb([128, 2, 128], BF16)
    vnb = p.sb([128, 2, 128], BF16)
    R = p.sb([128, 2, 128], F32)
    Rb = p.sb([128, 2, 128], BF16)
    S = p.sb([128, 2, 128], F32)
    Sb = p.sb([128, 2, 128], BF16)
    Snb = p.sb([128, 2, 128], BF16)
    bst = p.sb([128, 2, 6], F32)
    mv = p.sb([128, 2, 2], F32)
    st4 = p.sb([128, 8], F32)
    y1 = p.sb([128, 512], F32)
    y2 = p.sb([128, 512], F32)
    mixo = [p.sb([128, 512], BF16) for _ in range(2)]

    Bb = [p.ps([128, 1024], BF16) for _ in range(2)]
    Bk = [None, None] + [p.ps([128, 512], F32) for _ in range(6)]

    def q4(b, a, n=1):
        return Bk[b][:, a * 128:(a + n) * 128].rearrange("p (a c) -> p a c", a=n), ["B%d.%d" % (b, a + t) for t in range(n)]

    def _bk(keys):
        return [k.split(".")[0] if (k[0] == "B" and k[1].isdigit()) else k for k in keys]

    def OP(q, fn, r, w):
        p.op(q, fn, reads=_bk(r), writes=_bk(w))

    def MM(out, lhsT, rhs, st, sp, r, w):
        OP("pe", lambda e: e.matmul(out, lhsT, rhs, start=st, stop=sp), r, w)

    def TR(out, in_, ident, r, w):
        OP("pe", lambda e: e.transpose(out, in_, ident), r, w)

    def ACT(out, in_, func, r, w, **kw):
        OP("act", lambda e: e.activation(out, in_, func, **kw), r, w)

    def TS(q, out, in0, s1, s2, op0, op1, r, w):
        if op1 is None:
            OP(q, lambda e: e.tensor_scalar(out, in0, s1, None, op0), r, w)
        else:
            OP(q, lambda e: e.tensor_scalar(out, in0, s1, s2, op0, op1), r, w)

    def TT(q, out, in0, in1, op, r, w):
        OP(q, lambda e: e.tensor_tensor(out, in0, in1, op), r, w)

    def STT(out, in0, sc, in1, op0, op1, r, w):
        OP("dve", lambda e: e.scalar_tensor_tensor(out, in0, sc, in1, op0, op1), r, w)

    def CP(q, out, in_, r, w):
        if q == "act":
            OP(q, lambda e: e.copy(out, in_), r, w)
        else:
            OP(q, lambda e: e.tensor_copy(out, in_), r, w)

    for nm, t in [("nw", nw), ("convw", convw), ("smallc", smallc), ("gnw", gnw), ("dnw", dnw), ("identb", identb),
                  ("cf", cf), ("mask2", mask2), ("invf8", invf8), ("phase8", phase8), ("posT", posi)]:
        p.dma("sp", t[:], di[nm], writes=[nm])
    CP("dve", posf[:], posi[:], ["posT"], ["posf"])
    ACT(nea[:], alog, AF.Exp, ["smallc"], ["nea"])
    TS("dve", nea[:], nea[:], -1.0, None, ALU.mult, None, ["nea"], ["nea"])
    wt_v = di["w_tok"].rearrange("(k p) n -> p k n", p=128)
    wf_v = di["w_feat"].rearrange("(k p) n -> p k n", p=128)
    for kk in range(8):
        p.dma("sp", stg[:], wt_v[:, 2 * kk:2 * kk + 2, :], writes=["stg"])
        for t in range(2):
            k = 2 * kk + t
            TS("dve" if t == 0 else "pool", wtok[:, k, :], stg[:, t, :], nw[:, k:k + 1], None, ALU.mult, None,
               ["stg", "nw"], ["wtok"])
    for kk in range(8):
        p.dma("sp", stg[:, :, 0:NF], wf_v[:, 2 * kk:2 * kk + 2, :], writes=["stg"])
        for t in range(2):
            k = 2 * kk + t
            TS("dve" if t == 0 else "pool", wfeat[:, k, :], stg[:, t, 0:NF], nw[:, k:k + 1], None, ALU.mult, None,
               ["stg", "nw"], ["wfeat"])
    for m in range(24):
        TS("pool", dg[:, m, :], identf, convw[:, m:m + 1], None, ALU.mult, None, ["cf", "convw"], ["dg"])
    for t, nm in [(R, "R"), (S, "S")]:
        OP("pool", lambda e, t=t: e.memset(t[:], 0.0), [], [nm])
    for t, nm in [(Rb, "Rb"), (Sb, "Sb"), (Snb, "Snb"), (xc[1], "xc1"), (xc[0], "xc0")]:
        OP("pool", lambda e, t=t: e.memset(t[:], 0.0), [], [nm])

    for c in range(NCH):
        s = c % 2
        XS, XC, XCp, MO = "xs%d" % s, "xc%d" % s, "xc%d" % (1 - s), "mixo%d" % s
        p.dma("sp", xs[s][:], di["x"][c * 128:(c + 1) * 128, :], writes=[XS])
        TS("pool", ang[0][:], invf8[:], posf[:, c:c + 1], None, ALU.mult, None, ["invf8", "posf"], ["ang0"])
        TT("pool", ang[0][:], ang[0][:], phase8[:], ALU.add, ["ang0", "phase8"], ["ang0"])
        TS("pool", ang[1][:], ang[0][:], 1.0 / TWO_PI, None, ALU.mult, None, ["ang0"], ["ang1"])
        CP("pool", angi[:], ang[1][:], ["ang1"], ["angi"])
        CP("pool", ang[1][:], angi[:], ["angi"], ["ang1"])
        TS("pool", ang[2][:], ang[1][:], -C1, None, ALU.mult, None, ["ang1"], ["ang2"])
        TT("pool", ang[2][:], ang[2][:], ang[0][:], ALU.add, ["ang2", "ang0"], ["ang2"])
        TS("pool", ang[3][:], ang[1][:], -C2, None, ALU.mult, None, ["ang1"], ["ang3"])
        TT("pool", ang[2][:], ang[2][:], ang[3][:], ALU.add, ["ang2", "ang3"], ["ang2"])
        TS("pool", ang[3][:], ang[2][:], math.pi, -TWO_PI, ALU.is_gt, ALU.mult, ["ang2"], ["ang3"])
        TT("pool", ang[2][:], ang[2][:], ang[3][:], ALU.add, ["ang2", "ang3"], ["ang2"])
        TS("pool", ang[3][:], ang[2][:], -math.pi, TWO_PI, ALU.is_lt, ALU.mult, ["ang2"], ["ang3"])
        TT("pool", ang[2][:], ang[2][:], ang[3][:], ALU.add, ["ang2", "ang3"], ["ang2"])
        TS("pool", ang[2][:], ang[2][:], math.pi, -math.pi, ALU.min, ALU.max, ["ang2"], ["ang2"])

        ACT(ub[:], xs[s][:], AF.Square, [XS], ["ub", "st1"], accum_out=st1[:, 0:1])
        ACT(st1[:, 1:2], st1[:, 0:1], AF.Ln, ["st1"], ["st1"], scale=1.0 / D, bias=EPS)
        ACT(st1[:, 2:3], st1[:, 1:2], AF.Exp, ["st1"], ["st1"], scale=-0.5)
        ACT(ub[:], xs[s][:], AF.Copy, [XS, "st1"], ["ub"], scale=st1[:, 2:3])
        for k in range(16):
            TR(Bb[k // 8][:, (k % 8) * 128:(k % 8 + 1) * 128], ub[:, k * 128:(k + 1) * 128], identb[:],
               ["ub", "identb"], ["Bb%d" % (k // 8)])
        CP("dve", uT[:, 0:8, :], Bb[0][:].rearrange("p (a c) -> p a c", a=8), ["Bb0"], ["uT"])
        CP("act", uT[:, 8:16, :], Bb[1][:].rearrange("p (a c) -> p a c", a=8), ["Bb1"], ["uT"])
        for g, (b, lo, n) in enumerate([(2, 0, 512), (3, 512, 512), (4, 1024, 260)]):
            keys = ["B%d.%d" % (b, t) for t in range(4)]
            for k in range(16):
                MM(Bk[b][:, 0:n], uT[:, k, :], wtok[:, k, lo:lo + n], k == 0, k == 15, ["uT", "wtok"], keys)
        for m in range(6):
            b, a = (5, m) if m < 4 else (6, m - 4)
            for k in range(16):
                MM(Bk[b][:, a * 128:(a + 1) * 128], wfeat[:, k, m * 128:(m + 1) * 128], uT[:, k, :], k == 0, k == 15,
                   ["uT", "wfeat"], ["B%d.%d" % (b, a)])
        CP("act", qk[:], Bk[2][:], ["B2.0", "B2.1", "B2.2", "B2.3"], ["qk"])
        CP("act", xc[s][:, 0:4, 4:132], Bk[5][:].rearrange("p (a c) -> p a c", a=4), ["B5.0", "B5.1", "B5.2", "B5.3"], [XC])
        CP("act", xc[s][:, 4:6, 4:132], Bk[6][:, 0:256].rearrange("p (a c) -> p a c", a=2), ["B6.0", "B6.1"], [XC])
        CP("act", vb[:], Bk[3][:, 0:256], ["B3.0", "B3.1"], ["vb"])
        if c > 0:
            CP("pool", xc[s][:, :, 0:4], xc[1 - s][:, :, 128:132], [XCp], [XC])
        ACT(sc8[:], ang[2][:], AF.Sin, ["ang2"], ["sc8"])
        ACT(gs[:], Bk[3][:, 256:512], AF.Silu, ["B3.2", "B3.3"], ["gs"])
        ACT(zs[:], Bk[4][:, 0:256], AF.Silu, ["B4.0", "B4.1"], ["zs"])
        for m in range(6):
            b, a = (7, m) if m < 4 else (5, m - 4)
            for i in range(4):
                MM(Bk[b][:, a * 128:(a + 1) * 128], dg[:, m * 4 + i, :], xc[s][:, m, i + 1:i + 129], i == 0, i == 3,
                   [XC, "dg"], ["B%d.%d" % (b, a)])
        ACT(cT[:, 0:4, :], Bk[7][:].rearrange("p (a c) -> p a c", a=4), AF.Silu, ["B7.0", "B7.1", "B7.2", "B7.3"], ["cT"])
        ACT(cT[:, 4:6, :], Bk[5][:, 0:256].rearrange("p (a c) -> p a c", a=2), AF.Silu, ["B5.0", "B5.1"], ["cT"])
        TT("dve", st3[:, 0:2], Bk[4][:, 258:260], dtb, ALU.add, ["B4.2", "smallc"], ["st3a"])
        TS("dve", st3[:, 2:4], Bk[4][:, 256:258], -1.0, None, ALU.mult, None, ["B4.2"], ["st3b"])
        ACT(st3[:, 0:2], st3[:, 0:2], AF.Exp, ["st3a"], ["st3a"])
        ACT(st3[:, 2:4], st3[:, 2:4], AF.Exp, ["st3b"], ["st3b"])
        ACT(st3[:, 0:2], st3[:, 0:2], AF.Ln, ["st3a"], ["st3a"], bias=1.0)
        TS("dve", st3[:, 2:4], st3[:, 2:4], 1.0, None, ALU.add, None, ["st3b"], ["st3b"])
        OP("dve", lambda e: e.reciprocal(st3[:, 2:4], st3[:, 2:4]), ["st3b"], ["st3b"])
        TT("dve", st3[:, 4:6], st3[:, 0:2], nea[:], ALU.mult, ["st3a", "nea"], ["g"])
        qk4 = qk[:].rearrange("p (a b c) -> p a b c", a=4, b=2)
        rot4 = rot[:].rearrange("p (a b c) -> p a b c", a=4, b=2)
        sc4 = sc8[:].rearrange("p (t a c) -> p t a c", t=2, a=4)
        sn4, cs4 = sc4[:, 0], sc4[:, 1]
        tv = [t[:].rearrange("p (a c) -> p a c", a=4) for t in tt]
        TT("dve", tv[0], qk4[:, :, 0, :], cs4, ALU.mult, ["qk", "sc8"], ["tt0"])
        TT("pool", tv[1], qk4[:, :, 1, :], sn4, ALU.mult, ["qk", "sc8"], ["tt1"])
        TT("dve", rot4[:, :, 0, :], tv[0], tv[1], ALU.subtract, ["tt0", "tt1"], ["rot"])
        TT("pool", tv[2], qk4[:, :, 0, :], sn4, ALU.mult, ["qk", "sc8"], ["tt2"])
        TT("dve", tv[3], qk4[:, :, 1, :], cs4, ALU.mult, ["qk", "sc8"], ["tt3"])
        TT("pool", rot4[:, :, 1, :], tv[2], tv[3], ALU.add, ["tt2", "tt3"], ["rot"])
        for h in range(2):
            TS("dve", rb[:, h, :], rot[:, h * 128:(h + 1) * 128], dec(h, 0), None, ALU.mult, None, ["rot", "smallc"], ["rb"])
            TS("pool", rb[:, 2 + h, :], rot[:, (2 + h) * 128:(3 + h) * 128], dec(h, 1), None, ALU.mult, None, ["rot", "smallc"], ["rb"])
            TS("pool", rb[:, 4 + h, :], rot[:, (2 + h) * 128:(3 + h) * 128], dec(h, 2), None, ALU.mult, None, ["rot", "smallc"], ["rb"])
        for m in range(6):
            TR(Bb[0][:, m * 128:(m + 1) * 128], cT[:, m, :], identb[:], ["cT", "identb"], ["Bb0"])
        CP("dve", tmb[:], Bb[0][:, 0:768].rearrange("p (a c) -> p a c", a=6), ["Bb0"], ["tmb"])
        for m in range(4):
            TR(Bb[1][:, m * 128:(m + 1) * 128], rb[:, m, :], identb[:], ["rb", "identb"], ["Bb1"])
        CP("act", rT[:], Bb[1][:, 0:512].rearrange("p (a c) -> p a c", a=4), ["Bb1"], ["rT"])
        for h in range(2):
            MM(Bk[2][:, h * 128:(h + 1) * 128], rT[:, 2 + h, :], rT[:, h, :], True, True, ["rT"], ["B2.%d" % h])
        TT("dve", sTm[:], Bk[2][:, 0:256], mask2[:], ALU.mult, ["B2.0", "B2.1", "mask2"], ["sTm"])
        for h in range(2):
            MM(Bk[2][:, (2 + h) * 128:(3 + h) * 128], sTm[:, h * 128:(h + 1) * 128], vb[:, h * 128:(h + 1) * 128], True, False,
               ["sTm", "vb"], ["B2.%d" % (2 + h)])
            MM(Bk[2][:, (2 + h) * 128:(3 + h) * 128], rT[:, h, :], Rb[:, h, :], False, True, ["rT", "Rb"], ["B2.%d" % (2 + h)])
        for h in range(2):
            MM(Bk[3][:, h * 128:(h + 1) * 128], rb[:, 4 + h, :], vb[:, h * 128:(h + 1) * 128], True, True, ["rb", "vb"], ["B3.%d" % h])
        for h in range(2):
            STT(R[:, h, :], R[:, h, :], cdec[:, h:h + 1], Bk[3][:, h * 128:(h + 1) * 128], ALU.mult, ALU.add,
                ["R", "smallc", "B3.%d" % h], ["R"])
        CP("act", Rb[:], R[:], ["R"], ["Rb"])
        for h in range(2):
            OP("dve", lambda e, h=h: e.bn_stats(bst[:, h, :], Bk[2][:, (2 + h) * 128:(3 + h) * 128]), ["B2.%d" % (2 + h)], ["bst"])
            OP("dve", lambda e, h=h: e.bn_aggr(mv[:, h, :], bst[:, h, :]), ["bst"], ["mv"])
        mvv = mv[:].rearrange("p a b -> p (a b)")
        ACT(st4[:, 0:4], mvv, AF.Ln, ["mv"], ["st4"], bias=EPS)
        ACT(st4[:, 0:4], st4[:, 0:4], AF.Exp, ["st4"], ["st4"], scale=-0.5)
        for h in range(2):
            TS("dve", y1[:, h * 128:(h + 1) * 128], Bk[2][:, (2 + h) * 128:(3 + h) * 128], mv[:, h, 0:1], st4[:, 2 * h + 1:2 * h + 2],
               ALU.subtract, ALU.mult, ["B2.%d" % (2 + h), "mv", "st4"], ["y1r"])
        TT("pool", y2[:, 0:256], y1[:, 0:256], gnw[:], ALU.mult, ["y1r", "gnw"], ["y2r"])
        TT("pool", mixo[s][:, 0:256], y2[:, 0:256], gs[:], ALU.mult, ["y2r", "gs"], [MO])

        TT("dve", sq[:], tmb[:, 0:4, :], tmb[:, 0:4, :], ALU.mult, ["tmb"], ["sq"])
        OP("dve", lambda e: e.tensor_reduce(st2[:, 0:4], sq[:], mybir.AxisListType.X, ALU.add), ["sq"], ["st2"])
        ACT(st2[:, 4:8], st2[:, 0:4], AF.Ln, ["st2"], ["st2"], bias=EPS)
        ACT(st2[:, 8:12], st2[:, 4:8], AF.Exp, ["st2"], ["st2"], scale=-0.5)
        MM(Bk[4][:, 384:386], maskT, st3[:, 4:6], True, True, ["cf", "g"], ["B4.3"])
        MM(Bk[4][:, 386:388], onesf, st3[:, 4:6], True, True, ["cf", "g"], ["B4.3"])
        CP("dve", st3[:, 6:10], Bk[4][:, 384:388], ["B4.3"], ["gc"])
        TT("dve", st3[:, 10:12], st3[:, 8:10], st3[:, 6:8], ALU.subtract, ["gc"], ["gc"])
        ACT(st3[:, 12:18], st3[:, 6:12], AF.Exp, ["gc"], ["egc"])
        for h in range(2):
            TS("pool", GU[:, h, :], maskT, st3[:, 4 + h:5 + h], None, ALU.mult, None, ["cf", "g"], ["GU"])
            TS("pool", GUn[:, h, :], maskT, st3[:, 4 + h:5 + h], -1.0, ALU.mult, ALU.mult, ["cf", "g"], ["GUn"])
        for h in range(2):
            o_ = Bk[4][:, h * 128:(h + 1) * 128]
            MM(o_, onesf, GU[:, h, :], True, True, ["cf", "GU"], ["B4.%d" % h])
        for h in range(2):
            STT(gamT[:, h, :], Bk[4][:, h * 128:(h + 1) * 128], st3[:, 6 + h:7 + h], negT, ALU.subtract, ALU.add,
                ["B4.%d" % h, "gc", "cf"], ["gamT"])
        ACT(gamT[:], gamT[:], AF.Exp, ["gamT"], ["gamT"])
        for h in range(2):
            TT("pool", gamTs[:, h, :], gamT[:, h, :], strictT, ALU.mult, ["gamT", "cf"], ["gamTs"])
        for h in range(2):
            TS("dve", knb[:, h, :], tmb[:, 2 + h, :], st2[:, 10 + h:11 + h], None, ALU.mult, None, ["tmb", "st2"], ["knb"])
            TS("dve", qnb[:, h, :], tmb[:, h, :], st2[:, 8 + h:9 + h], 128 ** -0.5, ALU.mult, ALU.mult, ["tmb", "st2"], ["qnb"])
            TS("pool", qgb[:, h, :], qnb[:, h, :], st3[:, 12 + h:13 + h], None, ALU.mult, None, ["qnb", "egc"], ["qgb"])
            TS("pool", kegb[:, h, :], knb[:, h, :], st3[:, 12 + h:13 + h], None, ALU.mult, None, ["knb", "egc"], ["kegb"])
            TS("pool", kdb[:, h, :], knb[:, h, :], st3[:, 16 + h:17 + h], None, ALU.mult, None, ["knb", "egc"], ["kdb"])
        for m, src in enumerate([knb[:, 0, :], knb[:, 1, :], qnb[:, 0, :], qnb[:, 1, :], qgb[:, 0, :], qgb[:, 1, :]]):
            TR(Bb[1][:, m * 128:(m + 1) * 128], src, identb[:], ["knb", "qnb", "qgb", "identb"], ["Bb1"])
        CP("act", T2[:], Bb[1][:, 0:768].rearrange("p (a c) -> p a c", a=6), ["Bb1"], ["T2"])
        for h in range(2):
            MM(Bk[3][:, (2 + h) * 128:(3 + h) * 128], T2[:, h, :], T2[:, h, :], True, True, ["T2"], ["B3.%d" % (2 + h)])
            MM(Bk[4][:, (2 + h) * 128:(3 + h) * 128], T2[:, h, :], T2[:, 2 + h, :], True, True, ["T2"], ["B4.%d" % (2 + h)])
        for h in range(2):
            STT(Nf[0][:, h, :], Bk[3][:, (2 + h) * 128:(3 + h) * 128], st3[:, 2 + h:3 + h], gamTs[:, h, :], ALU.mult, ALU.mult,
                ["B3.%d" % (2 + h), "st3b", "gamTs"], ["N0"])
            TT("dve", qkTm[:, h, :], Bk[4][:, (2 + h) * 128:(3 + h) * 128], gamT[:, h, :], ALU.mult, ["B4.%d" % (2 + h), "gamT"], ["qkTm"])
        for h in range(2):
            TR(Bk[5][:, (2 + h) * 128:(3 + h) * 128], Nf[0][:, h, :], identf, ["N0", "cf"], ["B5.%d" % (2 + h)])
        CP("act", Bf[0][:], Bk[5][:, 256:512].rearrange("p (a c) -> p a c", a=2), ["B5.2", "B5.3"], ["Bf0"])
        for h in range(2):
            TT("pool", Qf[0][:, h, :], identf, Nf[0][:, h, :], ALU.subtract, ["cf", "N0"], ["Q0"])
        cur = 0
        PB = 3
        for l in range(1, 7):
            nx = 1 - cur
            Ncur, Bcur, Qcur = "N%d" % cur, "Bf%d" % cur, "Q%d" % cur
            Nnx, Bnx, Qnx = "N%d" % nx, "Bf%d" % nx, "Q%d" % nx
            for h in range(2):
                if l < 6:
                    MM(Bk[5][:, h * 128:(h + 1) * 128], Bf[cur][:, h, :], Nf[cur][:, h, :], True, True, [Ncur, Bcur], ["B5.%d" % h])
                MM(Bk[PB][:, (2 + h) * 128:(3 + h) * 128], Nf[cur][:, h, :], Bf[cur][:, h, :], True, True, [Ncur, Bcur], ["B%d.%d" % (PB, 2 + h)])
            if l < 6:
                CP("dve", Nf[nx][:], Bk[5][:, 0:256].rearrange("p (a c) -> p a c", a=2), ["B5.0", "B5.1"], [Nnx])
            CP("act", Bf[nx][:], Bk[PB][:, 256:512].rearrange("p (a c) -> p a c", a=2), ["B%d.2" % PB, "B%d.3" % PB], [Bnx])
            for h in range(2):
                o_ = Bk[6][:, h * 128:(h + 1) * 128]
                MM(o_, identf, Qf[cur][:, h, :], True, False, ["cf", Qcur], ["B6.%d" % h])
                MM(o_, Bf[nx][:, h, :], Qf[cur][:, h, :], False, True, [Bnx, Qcur], ["B6.%d" % h])
            if l < 6:
                CP("dve", Qf[nx][:], Bk[6][:, 0:256].rearrange("p (a c) -> p a c", a=2), ["B6.0", "B6.1"], [Qnx])
            else:
                CP("dve", Qb[:], Bk[6][:, 0:256].rearrange("p (a c) -> p a c", a=2), ["B6.0", "B6.1"], ["Qb"])
            cur = nx
        for h in range(2):
            MM(Bk[6][:, (2 + h) * 128:(3 + h) * 128], kegb[:, h, :], Qb[:, h, :], True, True, ["kegb", "Qb"], ["B6.%d" % (2 + h)])
        CP("act", wTb[:], Bk[6][:, 256:512].rearrange("p (a c) -> p a c", a=2), ["B6.2", "B6.3"], ["wTb"])
        for h in range(2):
            o_ = Bk[7][:, h * 128:(h + 1) * 128]
            MM(o_, Qb[:, h, :], tmb[:, 4 + h, :], True, False, ["Qb", "tmb"], ["B7.%d" % h])
            MM(o_, wTb[:, h, :], Snb[:, h, :], False, True, ["wTb", "Snb"], ["B7.%d" % h])
        for h in range(2):
            TS("dve", vnb[:, h, :], Bk[7][:, h * 128:(h + 1) * 128], st3[:, 2 + h:3 + h], None, ALU.mult, None,
               ["B7.%d" % h, "st3b"], ["vnb"])
        for h in range(2):
            o_ = Bk[7][:, (2 + h) * 128:(3 + h) * 128]
            MM(o_, T2[:, 4 + h, :], Sb[:, h, :], True, False, ["T2", "Sb"], ["B7.%d" % (2 + h)])
            MM(o_, qkTm[:, h, :], vnb[:, h, :], False, True, ["qkTm", "vnb"], ["B7.%d" % (2 + h)])
        for h in range(2):
            MM(Bk[3][:, (2 + h) * 128:(3 + h) * 128], kdb[:, h, :], vnb[:, h, :], True, True, ["kdb", "vnb"], ["B3.%d" % (2 + h)])
        for h in range(2):
            STT(S[:, h, :], S[:, h, :], st3[:, 14 + h:15 + h], Bk[3][:, (2 + h) * 128:(3 + h) * 128], ALU.mult, ALU.add,
                ["S", "egc", "B3.%d" % (2 + h)], ["S"])
        CP("act", Sb[:], S[:], ["S"], ["Sb"])
        ACT(Snb[:], S[:], AF.Copy, ["S"], ["Snb"], scale=-1.0)
        for h in range(2):
            ACT(y2[:, 256 + h * 128:384 + h * 128], Bk[7][:, (2 + h) * 128:(3 + h) * 128], AF.Square,
                ["B7.%d" % (2 + h)], ["y2g", "st4g"], accum_out=st4[:, 4 + h:5 + h])
        ACT(st4[:, 6:8], st4[:, 4:6], AF.Ln, ["st4g"], ["st4g"], scale=1.0 / 128, bias=EPS)
        ACT(st4[:, 6:8], st4[:, 6:8], AF.Exp, ["st4g"], ["st4g"], scale=-0.5)
        for h in range(2):
            STT(y1[:, 256 + h * 128:384 + h * 128], Bk[7][:, (2 + h) * 128:(3 + h) * 128], st4[:, 6 + h:7 + h], dnw[:],
                ALU.mult, ALU.mult, ["B7.%d" % (2 + h), "st4g", "dnw"], ["y1g"])
        TT("pool", mixo[s][:, 256:512], y1[:, 256:512], zs[:], ALU.mult, ["y1g", "zs"], [MO])
        if getattr(p, "dbg", None) is not None:
            TS("dve", mixo[s][:], mixo[s][:], -1e30, 1e30, ALU.max, ALU.min, [MO], [MO])
        p.dma("sp", mix_out[c * 128:(c + 1) * 128, :], mixo[s][:], reads=[MO], writes=["mix_out"])
        if getattr(p, "dbg", None) is not None and c == 0:
            dbt = p.sb([128, 8, 128], F32)
            OP("pool", lambda e: e.memset(dbt[:], 0.0), [], ["dbt"])
            srcs = [(0, 2, Nf[0], "N0"), (2, 4, Bf[0], "Bf0"), (4, 6, gamT, "gamT")]
            for a, b, t, k in srcs:
                TS("dve", dbt[:, a:b, :], t[:], -1e30, 1e30, ALU.max, ALU.min, [k, "dbt"], ["dbt"])
            TS("dve", dbt[:, 6, 0:32], st3[:], -1e30, 1e30, ALU.max, ALU.min, ["g", "gc", "egc", "st3a", "st3b", "dbt"], ["dbt"])
            TS("dve", dbt[:, 6, 32:48], st2[:], -1e30, 1e30, ALU.max, ALU.min, ["st2", "dbt"], ["dbt"])
            TS("dve", dbt[:, 7, :], y1[:, 256:384], -1e30, 1e30, ALU.max, ALU.min, ["y1g", "dbt"], ["dbt"])
            p.dma("sp", p.dbg, dbt[:], reads=["dbt"], writes=["dbg"])


NE = 32
DFF = 1024
EPS_B = 1e-6


def b_inputs(nc, TB):
    di = {}

    def inp(name, shape, dt):
        di[name] = nc.dram_tensor(name, list(shape), dt, kind="ExternalInput").ap()

    inp("xT", [D, TB], F32)
    inp("mixT", [D, TB], BF16)
    inp("memT", [D, 256], F32)
    inp("w_out", [D, D], F32)
    inp("w_xq", [D, D], F32)
    inp("w_xkv", [D, 2 * D], F32)
    inp("w_xo", [D, D], F32)
    inp("w_r", [D, 36], F32)
    inp("b_r", [128, 4, 36], F32)
    inp("w_gate", [NE, D, DFF], F32)
    inp("w_up", [NE, D, DFF], F32)
    inp("w_down", [NE, DFF, D], F32)
    inp("nws", [128, 4, 16], F32)
    inp("onesb", [128, 128], BF16)
    inp("identf", [128, 128], F32)
    inp("identb", [128, 128], BF16)
    inp("sel", [32, NE, 128], F32)
    return di


def build_B(p, di, outT, TB):
    NS = TB // 512
    hT = p.sb([128, 16, 512], F32)
    actA = p.sb([128, 16, 512], BF16)
    actB = p.sb([128, 16, 512], BF16)
    hid = p.sb([128, 8, 512], BF16)
    stg = [p.sb([128, 16, 256], F32) for _ in range(2)]
    wbf = [p.sb([128, 16, 256], BF16) for _ in range(2)]
    KT = p.sb([128, 16, 256], BF16)
    Vt = p.sb([128, 2, D], BF16)
    ET = p.sb([128, 2, 512], BF16)
    rs = p.sb([128, 512], F32)
    tmpf = p.sb([128, 512], F32)
    tmp2 = p.sb([128, 512], F32)
    gbc = p.sb([128, 512], F32)
    GT = p.sb([32, 512], F32)
    lgT = p.sb([36, 512], F32)
    lg = p.sb([128, 4, 36], F32)
    Gt = p.sb([128, 4, 32], F32)
    rt = p.sb([128, 64], F32)
    rv = p.sb([128, 4, 32], F32)
    nws = p.sb([128, 4, 16], F32)
    b_r = p.sb([128, 4, 36], F32)
    onesb = p.sb([128, 128], BF16)
    identf = p.sb([128, 128], F32)
    identb = p.sb([128, 128], BF16)
    sel = p.sb([32, NE, 128], F32)
    wr = p.sb([128, 16, 36], F32)
    wrh = p.sb([128, 16, 36], BF16)
    wrl = p.sb([128, 16, 36], BF16)
    ps = [p.ps([128, 512], F32) for _ in range(4)]
    psD = p.ps([128, 512], F32)
    psS = [p.ps([128, 512], F32) for _ in range(2)]
    psT = p.ps([128, 1024], BF16)

    def OP(q, fn, r, w):
        p.op(q, fn, reads=r, writes=w)

    def MM(out, lhsT, rhs, st, sp, r, w):
        OP("pe", lambda e: e.matmul(out, lhsT, rhs, start=st, stop=sp), r, w)

    def ACT(out, in_, func, r, w, **kw):
        OP("act", lambda e: e.activation(out, in_, func, **kw), r, w)

    def TS(q, out, in0, s1, s2, op0, op1, r, w):
        if op1 is None:
            OP(q, lambda e: e.tensor_scalar(out, in0, s1, None, op0), r, w)
        else:
            OP(q, lambda e: e.tensor_scalar(out, in0, s1, s2, op0, op1), r, w)

    def TT(q, out, in0, in1, op, r, w):
        OP(q, lambda e: e.tensor_tensor(out, in0, in1, op), r, w)

    def STT(out, in0, sc, in1, op0, op1, r, w):
        OP("dve", lambda e: e.scalar_tensor_tensor(out, in0, sc, in1, op0, op1), r, w)

    def CP(q, out, in_, r, w):
        if q == "act":
            OP(q, lambda e: e.copy(out, in_), r, w)
        else:
            OP(q, lambda e: e.tensor_copy(out, in_), r, w)

    state = {"blk": 0, "ps": 0}

    def LIN(W, K, ncols, act, actkey, N, handler, col0=0):
        KTn = K // 128
        Wv = W.rearrange("(k p) n -> p k n", p=128)
        nblk = (ncols + 255) // 256
        for bi in range(nblk):
            c0 = bi * 256
            cw = min(256, ncols - c0)
            sl = state["blk"] % 2
            state["blk"] += 1
            SK, WK = "stg%d" % sl, "wbf%d" % sl
            p.dma("sp", stg[sl][:, 0:KTn, 0:cw], Wv[:, :, col0 + c0:col0 + c0 + cw], writes=[SK])
            h = KTn // 2
            CP("pool", wbf[sl][:, 0:h, 0:cw], stg[sl][:, 0:h, 0:cw], [SK], [WK])
            CP("dve", wbf[sl][:, h:KTn, 0:cw], stg[sl][:, h:KTn, 0:cw], [SK], [WK + "b"])
            for t in range(cw // 128):
                n = (c0 // 128) + t
                b = state["ps"] % 4
                state["ps"] += 1
                for k in range(KTn):
                    MM(ps[b][:, 0:N], wbf[sl][:, k, t * 128:(t + 1) * 128], act[:, k, 0:N], k == 0, k == KTn - 1,
                       [WK, WK + "b", actkey], ["ps%d" % b])
                handler(n, ps[b][:, 0:N], "ps%d" % b)

    def NORM(src, srckey, widx, out, outkey, N, lo=None, lokey=None, out_f32=False):
        for n in range(16):
            ACT(actB[:, n, 0:N], src[:, n, 0:N], AF.Square, [srckey], ["actB"])
        for k in range(16):
            MM(psD[:, 0:N], onesb[:], actB[:, k, 0:N], k == 0, k == 15, ["onesb", "actB"], ["psD"])
        ACT(rs[:, 0:N], psD[:, 0:N], AF.Ln, ["psD"], ["rs"], scale=1.0 / D, bias=EPS_B)
        ACT(rs[:, 0:N], rs[:, 0:N], AF.Exp, ["rs"], ["rs"], scale=-0.5)
        for n in range(16):
            if lo is None:
                STT(out[:, n, 0:N], src[:, n, 0:N], nws[:, widx, n:n + 1], rs[:, 0:N], ALU.mult, ALU.mult,
                    [srckey, "nws", "rs"], [outkey])
            else:
                STT(tmpf[:, 0:N], src[:, n, 0:N], nws[:, widx, n:n + 1], rs[:, 0:N], ALU.mult, ALU.mult,
                    [srckey, "nws", "rs"], ["tmpf"])
                CP("act", out[:, n, 0:N], tmpf[:, 0:N], ["tmpf"], [outkey])
                TT("dve", lo[:, n, 0:N], tmpf[:, 0:N], out[:, n, 0:N], ALU.subtract, ["tmpf", outkey], [lokey])

    for nm, t in [("nws", nws), ("b_r", b_r), ("onesb", onesb), ("identf", identf), ("identb", identb), ("sel", sel)]:
        p.dma("sp", t[:], di[nm], writes=[nm])
    p.dma("sp", wr[:], di["w_r"].rearrange("(k p) n -> p k n", p=128), writes=["wr"])
    CP("dve", wrh[:], wr[:], ["wr"], ["wrh"])
    TT("dve", wrl[:], wr[:], wrh[:], ALU.subtract, ["wr", "wrh"], ["wrl"])
    p.dma("sp", hT[:, :, 0:256], di["memT"].rearrange("(k p) t -> p k t", p=128), writes=["hT"])
    NORM(hT, "hT", 1, actA, "actA", 256)

    def h_k(n, pp, pk):
        CP("act", KT[:, n, :], pp, [pk], ["KT"])

    LIN(di["w_xkv"], D, D, actA, "actA", 256, h_k, col0=0)

    def h_v(n, pp, pk):
        CP("act", ET[:, 0, 0:256], pp, [pk], ["ET"])
        for mt in range(2):
            OP("pe", lambda e, mt=mt: e.transpose(psT[:, mt * 128:(mt + 1) * 128], ET[:, 0, mt * 128:(mt + 1) * 128], identb[:]),
               ["ET", "identb"], ["psT"])
        CP("dve", Vt[:, :, n * 128:(n + 1) * 128], psT[:, 0:256].rearrange("p (a c) -> p a c", a=2), ["psT"], ["Vt"])

    LIN(di["w_xkv"], D, D, actA, "actA", 256, h_v, col0=D)

    xv = di["xT"].rearrange("(k p) t -> p k t", p=128)
    mv = di["mixT"].rearrange("(k p) t -> p k t", p=128)
    ov = outT.rearrange("(k p) t -> p k t", p=128)

    def h_addh(n, pp, pk):
        TT("dve", hT[:, n, :], hT[:, n, :], pp, ALU.add, ["hT", pk], ["hT"])

    for sbi in range(NS):
        tsl = slice(sbi * 512, (sbi + 1) * 512)
        p.dma("sp", hT[:], xv[:, :, tsl], writes=["hT"])
        p.dma("sp", actA[:], mv[:, :, tsl], writes=["actA"])
        LIN(di["w_out"], D, D, actA, "actA", 512, h_addh)
        if getattr(p, "stage", "out") == "h1":
            p.dma("sp", ov[:, :, tsl], hT[:], reads=["hT"], writes=["outT"])
            continue
        NORM(hT, "hT", 0, actA, "actA", 512)

        def h_q(n, pp, pk):
            CP("act", actB[:, n, :], pp, [pk], ["actB"])

        LIN(di["w_xq"], D, D, actA, "actA", 512, h_q)
        for hd in range(4):
            for mt in range(2):
                for j in range(4):
                    MM(psS[mt][:], KT[:, 4 * hd + j, mt * 128:(mt + 1) * 128], actB[:, 4 * hd + j, :], j == 0, j == 3,
                       ["KT", "actB"], ["psS%d" % mt])
                ACT(ET[:, mt, :], psS[mt][:], AF.Exp, ["psS%d" % mt], ["ET"], scale=512 ** -0.5)
            for mt in range(2):
                MM(psD[:], onesb[:], ET[:, mt, :], mt == 0, mt == 1, ["onesb", "ET"], ["psD"])
            OP("dve", lambda e: e.reciprocal(rs[:], psD[:]), ["psD"], ["rs"])
            for j in range(4):
                b = state["ps"] % 4
                state["ps"] += 1
                n = 4 * hd + j
                for mt in range(2):
                    MM(ps[b][:], Vt[:, mt, n * 128:(n + 1) * 128], ET[:, mt, :], mt == 0, mt == 1, ["Vt", "ET"], ["ps%d" % b])
                TT("dve", actA[:, n, :], ps[b][:], rs[:], ALU.mult, ["ps%d" % b, "rs"], ["actA"])
        LIN(di["w_xo"], D, D, actA, "actA", 512, h_addh)
        if getattr(p, "stage", "out") == "h2":
            p.dma("sp", ov[:, :, tsl], hT[:], reads=["hT"], writes=["outT"])
            continue
        _norm_moe(p, OP, MM, ACT, STT, CP, TT, hT, actA, actB, onesb, psD, rs, tmpf, nws)
        combos = [(wrh, actA, "actA"), (wrh, actB, "actB"), (wrl, actA, "actA")]
        for ci, (wt, at, ak) in enumerate(combos):
            for k in range(16):
                MM(psD[0:36, :], wt[:, k, :], at[:, k, :], ci == 0 and k == 0, ci == 2 and k == 15, ["wrh", "wrl", ak], ["psD"])
        CP("act", lgT[:], psD[0:36, :], ["psD"], ["lgT"])
        for tt in range(4):
            OP("pe", lambda e, tt=tt: e.transpose(psS[0][:, tt * 36:(tt + 1) * 36], lgT[:, tt * 128:(tt + 1) * 128], identf[0:36, 0:36]),
               ["lgT", "identf"], ["psS0"])
        TT("dve", lg[:], psS[0][:, 0:144].rearrange("p (a c) -> p a c", a=4), b_r[:], ALU.add, ["psS0", "b_r"], ["lg"])
        def router_tile(tt):
            gl = lg[:, tt, 0:4]
            el = lg[:, tt, 4:36]
            R = lambda a, b=None: rt[:, tt * 16 + a: tt * 16 + (b if b is not None else a + 1)]
            lem, eq1, lem2, eq2 = rv[:, 0, :], rv[:, 1, :], rv[:, 2, :], rv[:, 3, :]
            RK = ["rt", "rv"]
            OP("dve", lambda e, gl=gl, R=R: e.tensor_reduce(R(0), gl, mybir.AxisListType.X, ALU.max), ["lg"], RK)
            TS("dve", R(1), R(0), -1.0, None, ALU.mult, None, RK, RK)
            ACT(R(4, 8), gl, AF.Exp, ["lg"] + RK, RK, bias=R(1), accum_out=R(2))
            OP("dve", lambda e, R=R: e.reciprocal(R(3), R(2)), RK, RK)
            TS("dve", R(4, 8), gl, R(0), None, ALU.is_equal, None, ["lg"] + RK, RK)
            TS("dve", R(8, 12), R(4, 8), -1.0, 1e30, ALU.add, ALU.mult, RK, RK)
            for g in range(4):
                TS("dve", rv[:, 0, g * 8:(g + 1) * 8], lg[:, tt, 4 + g * 8:12 + g * 8], R(8 + g), None, ALU.add, None, ["lg"] + RK, RK)
            OP("dve", lambda e, R=R, lem=lem: e.tensor_reduce(R(12), lem, mybir.AxisListType.X, ALU.max), RK, RK)
            TS("dve", eq1, lem, R(12), None, ALU.is_equal, None, RK, RK)
            STT(lem2, eq1, -1e30, lem, ALU.mult, ALU.add, RK, RK)
            OP("dve", lambda e, R=R, lem2=lem2: e.tensor_reduce(R(13), lem2, mybir.AxisListType.X, ALU.max), RK, RK)
            TS("dve", eq2, lem2, R(13), None, ALU.is_equal, None, RK, RK)
            TT("dve", R(14), R(13), R(12), ALU.subtract, RK, RK)
            ACT(R(14), R(14), AF.Exp, RK, RK)
            TS("dve", R(15), R(14), 1.0, None, ALU.add, None, RK, RK)
            OP("dve", lambda e, R=R: e.reciprocal(R(15), R(15)), RK, RK)
            TT("dve", R(15), R(15), R(3), ALU.mult, RK, RK)
            TT("dve", R(14), R(14), R(15), ALU.mult, RK, RK)
            TS("dve", Gt[:, tt, :], eq1, R(15), None, ALU.mult, None, RK, ["Gt"])
            STT(Gt[:, tt, :], eq2, R(14), Gt[:, tt, :], ALU.mult, ALU.add, RK + ["Gt"], ["Gt"])
        for tt in range(4):
            router_tile(tt)
        for tt in range(4):
            OP("pe", lambda e, tt=tt: e.transpose(psS[1][0:32, tt * 128:(tt + 1) * 128], Gt[:, tt, :], identf[:]),
               ["Gt", "identf"], ["psS1"])
        CP("act", GT[:], psS[1][0:32, :], ["psS1"], ["GT"])
        for ex in range(NE):
            MM(psD[:], sel[:, ex, :], GT[:], True, True, ["sel", "GT"], ["psD"])
            CP("act", gbc[:], psD[:], ["psD"], ["gbc"])
            Wg = di["w_gate"][ex].rearrange("(k p) n -> p k n", p=128)
            Wu = di["w_up"][ex].rearrange("(k p) n -> p k n", p=128)
            for fb in range(DFF // 256):
                keys = []
                for wi, Wv in enumerate([Wg, Wu]):
                    sl = state["blk"] % 2
                    state["blk"] += 1
                    SK, WK = "stg%d" % sl, "wbf%d" % sl
                    p.dma("sp", stg[sl][:], Wv[:, :, fb * 256:(fb + 1) * 256], writes=[SK])
                    CP("pool", wbf[sl][:, 0:8, :], stg[sl][:, 0:8, :], [SK], [WK])
                    CP("dve", wbf[sl][:, 8:16, :], stg[sl][:, 8:16, :], [SK], [WK + "b"])
                    keys.append((sl, WK))
                for t in range(2):
                    f = fb * 2 + t
                    bg, bu = (state["ps"] % 4), ((state["ps"] + 1) % 4)
                    state["ps"] += 2
                    for (sl, WK), b in zip(keys, (bg, bu)):
                        for k in range(16):
                            MM(ps[b][:], wbf[sl][:, k, t * 128:(t + 1) * 128], actA[:, k, :], k == 0, k == 15,
                               [WK, WK + "b", "actA"], ["ps%d" % b])
                    ACT(tmpf[:], ps[bg][:], AF.Silu, ["ps%d" % bg], ["tmpf"])
                    TT("dve", tmp2[:], tmpf[:], ps[bu][:], ALU.mult, ["tmpf", "ps%d" % bu], ["tmp2"])
                    TT("pool", hid[:, f, :], tmp2[:], gbc[:], ALU.mult, ["tmp2", "gbc"], ["hid"])
            LIN(di["w_down"][ex], DFF, D, hid, "hid", 512, h_addh)
        if getattr(p, "stage", "out") == "h3":
            p.dma("sp", ov[:, :, tsl], hT[:], reads=["hT"], writes=["outT"])
            continue
        for n in range(16):
            ACT(actB[:, n, :], hT[:, n, :], AF.Square, ["hT"], ["actB"])
        for k in range(16):
            MM(psD[:], onesb[:], actB[:, k, :], k == 0, k == 15, ["onesb", "actB"], ["psD"])
        ACT(rs[:], psD[:], AF.Ln, ["psD"], ["rs"], scale=1.0 / D, bias=EPS_B)
        ACT(rs[:], rs[:], AF.Exp, ["rs"], ["rs"], scale=-0.5)
        for n in range(16):
            STT(hT[:, n, :], hT[:, n, :], nws[:, 3, n:n + 1], rs[:], ALU.mult, ALU.mult, ["hT", "nws", "rs"], ["hT"])
        p.dma("sp", ov[:, :, tsl], hT[:], reads=["hT"], writes=["outT"])


def _norm_moe(p, OP, MM, ACT, STT, CP, TT, hT, actA, actB, onesb, psD, rs, tmpf, nws):
    N = 512
    for n in range(16):
        ACT(actB[:, n, :], hT[:, n, :], AF.Square, ["hT"], ["actB"])
    for k in range(16):
        MM(psD[:], onesb[:], actB[:, k, :], k == 0, k == 15, ["onesb", "actB"], ["psD"])
    ACT(rs[:], psD[:], AF.Ln, ["psD"], ["rs"], scale=1.0 / D, bias=EPS_B)
    ACT(rs[:], rs[:], AF.Exp, ["rs"], ["rs"], scale=-0.5)
    for n in range(16):
        STT(tmpf[:], hT[:, n, :], nws[:, 2, n:n + 1], rs[:], ALU.mult, ALU.mult, ["hT", "nws", "rs"], ["tmpf"])
        CP("act", actA[:, n, :], tmpf[:], ["tmpf"], ["actA"])
        TT("dve", actB[:, n, :], tmpf[:], actA[:, n, :], ALU.subtract, ["tmpf", "actA"], ["actB"])

def a_core_inputs(inp, b, hg, NCH):
    T = NCH * 128
    w_in = inp["w_in"][0]
    RQ, RV, DQ = 1024, 1024, 1024
    o_rq, o_rk, o_rv, o_rg = 0, 1024, 2048, 3072
    o_dqkv = 4096
    o_dz = 4096 + 3072
    o_db = o_dz + 1024
    o_da = o_db + 8
    hs = [2 * hg, 2 * hg + 1]

    def cols(base, h):
        return np.arange(base + h * 128, base + (h + 1) * 128)

    tok_cols = np.concatenate(
        [cols(o_rq, h) for h in hs] + [cols(o_rk, h) for h in hs] + [cols(o_rv, h) for h in hs] + [cols(o_rg, h) for h in hs]
        + [cols(o_dz, h) for h in hs] + [np.array([o_db + h for h in hs])] + [np.array([o_da + h for h in hs])])
    feat_cols = np.concatenate([cols(o_dqkv, h) for h in hs] + [cols(o_dqkv + 1024, h) for h in hs] + [cols(o_dqkv + 2048, h) for h in hs])
    d = {}
    d["x"] = np.ascontiguousarray(inp["x"][b, :T])
    d["posT"] = np.ascontiguousarray(inp["positions"][b, :T].reshape(NCH, 128).T)
    d["w_tok"] = np.ascontiguousarray(w_in[:, tok_cols])
    d["w_feat"] = np.ascontiguousarray(w_in[:, feat_cols])
    d["nw"] = np.ascontiguousarray(inp["norm_mix_w"][0].reshape(16, 128).T)
    cw = inp["dn_conv_w"][0][:, feat_cols - o_dqkv]
    d["convw"] = np.ascontiguousarray(cw.reshape(4, 6, 128).transpose(2, 1, 0).reshape(128, 24))
    c = a_host_consts(2 * hg, NCH)
    sm = c["smallc"].copy()
    sm[:, 0:2] = inp["dn_a_log"][0][hs][None, :]
    sm[:, 2:4] = inp["dn_dt_bias"][0][hs][None, :]
    c["smallc"] = sm
    d.update(c)
    d["gnw"] = np.ascontiguousarray(np.broadcast_to(inp["ret_gn_w"][0][2 * hg * 128:(2 * hg + 2) * 128][None, :], (128, 256)))
    d["dnw"] = np.ascontiguousarray(np.broadcast_to(inp["dn_norm_w"][0][None, :], (128, 128)))
    return d


def b_core_inputs(inp, mix, c, TB):
    b, sg = c // 4, c % 4
    sl = slice(sg * TB, (sg + 1) * TB)
    d = {}
    d["xT"] = np.ascontiguousarray(inp["x"][b, sl].T)
    d["mixT"] = np.ascontiguousarray(mix[b, sl].T)
    d["memT"] = np.ascontiguousarray(inp["mem"][b].T)
    d["w_out"] = inp["w_out"][0]
    d["w_xq"] = inp["w_xq"][0]
    d["w_xkv"] = inp["w_xkv"][0]
    d["w_xo"] = inp["w_xo"][0]
    d["w_r"] = np.ascontiguousarray(np.concatenate([inp["w_group_router"][0], inp["w_expert_router"][0]], axis=1))
    br = np.concatenate([inp["b_group_router"][0], inp["b_expert_router"][0]])
    d["b_r"] = np.ascontiguousarray(np.broadcast_to(br[None, None, :], (128, 4, 36))).astype(np.float32)
    d["w_gate"] = inp["w_gate"][0]
    d["w_up"] = inp["w_up"][0]
    d["w_down"] = inp["w_down"][0]
    nws = np.stack([inp["norm_xq_w"][0], inp["norm_mem_w"][0], inp["norm_moe_w"][0], inp["norm_final_w"]], axis=0)
    d["nws"] = np.ascontiguousarray(nws.reshape(4, 16, 128).transpose(2, 0, 1))
    d["onesb"] = np.ones((128, 128), dtype=ml_dtypes.bfloat16)
    d["identf"] = np.eye(128, dtype=np.float32)
    d["identb"] = np.eye(128, dtype=np.float32).astype(ml_dtypes.bfloat16)
    sel = np.zeros((32, 32, 128), np.float32)
    for e in range(32):
        sel[e, e, :] = 1.0
    d["sel"] = sel
    return d


def _build_a(NCH):
    nc = bass.Bass("TRN2", target_bir_lowering=False)
    di = a_inputs(nc, NCH)
    mix = nc.dram_tensor("mix", [NCH * 128, 512], BF16, kind="ExternalOutput").ap()
    p = Prog(nc)
    build_A(p, di, mix, NCH)
    p.wait_all("sp", ["mix_out"])
    p.emit()
    p.close()
    return nc


def _build_b(TB):
    nc = bass.Bass("TRN2", target_bir_lowering=False)
    di = b_inputs(nc, TB)
    outT = nc.dram_tensor("outT", [D, TB], F32, kind="ExternalOutput").ap()
    p = Prog(nc)
    build_B(p, di, outT, TB)
    p.wait_all("sp", ["outT"])
    p.emit()
    p.close()
    return nc


def kernel(**inputs):
    inp = {k: np.asarray(v) for k, v in inputs.items()}
    B, S, _ = inp["x"].shape
    NCH = S // 128
    nc = _build_a(NCH)
    maps = [a_core_inputs(inp, c // 4, c % 4, NCH) for c in range(8)]
    res = run_bass_kernel_spmd(nc, maps, core_ids=list(range(8)))
    mix = np.zeros((B, S, 2048), dtype=ml_dtypes.bfloat16)
    for c in range(8):
        b, hg = c // 4, c % 4
        m = res.results[c]["mix"]
        mix[b, :, hg * 256:(hg + 1) * 256] = m[:, 0:256]
        mix[b, :, 1024 + hg * 256:1024 + (hg + 1) * 256] = m[:, 256:512]
    del res, maps
    TB = S // 4
    ncb = _build_b(TB)
    mapsb = [b_core_inputs(inp, mix, c, TB) for c in range(8)]
    resb = run_bass_kernel_spmd(ncb, mapsb, core_ids=list(range(8)))
    out = np.zeros((B, S, 2048), np.float32)
    for c in range(8):
        b, sg = c // 4, c % 4
        out[b, sg * TB:(sg + 1) * TB] = resb.results[c]["outT"].T
    return out
```
